# Optimizing a Trainium2 kernel written in Bass

```python
import math
import jax, jax.numpy as jnp
from jax import lax
import numpy as np

D_MODEL = 1024
BATCH = 8
SEQ = 2048
DEPTH = 1

GRID_W = 64
CTX_LEN = 256
CONV_WIDTH = D_MODEL
CONV_K = 3
N_HEADS = 8
HEAD_DIM = 64
V_DIM = 2 * HEAD_DIM
ATTN_QK = N_HEADS * 2 * HEAD_DIM
ATTN_V = N_HEADS * V_DIM
N_EXPERTS = 16
EXPERT_HIDDEN = 2048
CAPACITY_FACTOR = 2
ROPE_THETA = 10000.0
Q_BLOCK = 128
EPS = 1e-6
PROJ_SIZES = (CONV_WIDTH, CONV_WIDTH, CONV_WIDTH, ATTN_QK, ATTN_QK, ATTN_V, D_MODEL, D_MODEL)
PROJ_COLS = sum(PROJ_SIZES)

kernel_name = 'hybrid_conv_diffattn_ec_moe_dit'


def rmsnorm(x, g):
    xf = x.astype(jnp.float32)
    y = xf * lax.rsqrt(jnp.mean(xf * xf, axis=-1, keepdims=True) + EPS)
    return (y * g.astype(jnp.float32)).astype(x.dtype)


def ada_mod(cond, w_ada, b_ada):
    m = jax.nn.silu(cond) @ w_ada + b_ada
    m = m.reshape(-1, 1, 6 * D_MODEL)
    return jnp.split(m, 6, axis=-1)


def modulate(h, shift, scale):
    return h * (1 + scale) + shift


def axial_rope_tables(n):
    rows = n // GRID_W
    row = jnp.repeat(jnp.arange(rows), GRID_W).astype(jnp.float32)
    col = jnp.tile(jnp.arange(GRID_W), rows).astype(jnp.float32)
    per_axis = HEAD_DIM // 2
    inv = ROPE_THETA ** (-jnp.arange(0, per_axis, 2, dtype=jnp.float32) / per_axis)
    ang = jnp.stack([row[:, None] * inv, col[:, None] * inv], axis=1)
    return jnp.cos(ang), jnp.sin(ang)


def apply_rope(x, cos, sin):
    shp = x.shape
    xr = x.astype(jnp.float32).reshape(shp[:-1] + (2, 2, HEAD_DIM // 4))
    x1, x2 = xr[..., 0, :], xr[..., 1, :]
    cs, sn = cos[:, None, None], sin[:, None, None]
    out = jnp.stack([x1 * cs - x2 * sn, x2 * cs + x1 * sn], axis=-2)
    return out.reshape(shp).astype(x.dtype)


def split_proj(p):
    idx = [int(v) for v in np.cumsum(PROJ_SIZES)[:-1]]
    return jnp.split(p, idx, axis=-1)


def attn_heads(parts):
    B, n, _ = parts[3].shape
    q = parts[3].reshape(B, n, N_HEADS, 2, HEAD_DIM)
    k = parts[4].reshape(B, n, N_HEADS, 2, HEAD_DIM)
    v = parts[5].reshape(B, n, N_HEADS, V_DIM).transpose(0, 2, 1, 3)
    return q, k, v


def to_bhm(t):
    return t.transpose(0, 2, 3, 1, 4)


def diff_attn(q, k, v, lam):
    s = jnp.einsum('bhmqd,bhmkd->bhmqk', q, k).astype(jnp.float32) * (HEAD_DIM ** -0.5)
    p = jax.nn.softmax(s, axis=-1)
    a = p[:, :, 0] - lam * p[:, :, 1]
    return jnp.einsum('bhqk,bhkv->bhqv', a.astype(v.dtype), v)


def blocked_diff_attn(q, k, v, lam):
    B, H, _, n, hd = q.shape
    nb = n // Q_BLOCK
    qb = q.reshape(B, H, 2, nb, Q_BLOCK, hd).transpose(3, 0, 1, 2, 4, 5)
    ob = lax.map(lambda qi: diff_attn(qi, k, v, lam), qb)
    return ob.transpose(1, 2, 0, 3, 4).reshape(B, H, n, V_DIM)


def short_conv_branch(u, gate_b, gate_c, conv_w, w_out_conv):
    z = gate_c * u
    zp = jnp.pad(z, ((0, 0), (1, 1), (0, 0)))
    y = zp[:, :-2] * conv_w[0] + zp[:, 1:-1] * conv_w[1] + zp[:, 2:] * conv_w[2]
    return (gate_b * y) @ w_out_conv


def merge_branches(parts, o, conv_w, w_out_conv, subln_g, lam_init, w_o_attn, w_out):
    y_conv = short_conv_branch(parts[0], parts[1], parts[2], conv_w, w_out_conv)
    B, H, n, _ = o.shape
    o = rmsnorm(o, subln_g) * (1.0 - lam_init)
    y_attn = o.transpose(0, 2, 1, 3).reshape(B, n, ATTN_V) @ w_o_attn
    merged = jax.nn.sigmoid(parts[6]) * y_conv + jax.nn.sigmoid(parts[7]) * y_attn
    return merged @ w_out


def expert_choice_ffn(h, w_router, w_gate, w_up, w_down):
    B, n, _ = h.shape
    cap = CAPACITY_FACTOR * n // N_EXPERTS
    aff = jax.nn.softmax((h @ w_router).astype(jnp.float32), axis=-1)
    g, idx = lax.top_k(aff.transpose(0, 2, 1), cap)
    bidx = jnp.arange(B)[:, None, None]
    xs = h[bidx, idx]
    a = jnp.einsum('becd,edf->becf', xs, w_gate)
    u = jnp.einsum('becd,edf->becf', xs, w_up)
    y = jnp.einsum('becf,efd->becd', jax.nn.silu(a) * u, w_down)
    y = y * g[..., None].astype(y.dtype)
    return jnp.zeros_like(h).at[bidx, idx].add(y)


def setup_inputs(seed: int = 0) -> dict:
    key = jax.random.key(seed)
    ks = jax.random.split(key, 32)
    L, D, F, E = DEPTH, D_MODEL, EXPERT_HIDDEN, N_EXPERTS

    def nrm(k, shape, scale):
        return jax.random.normal(k, shape, jnp.float32) * scale

    return {
        'x': nrm(ks[0], (BATCH, SEQ, D), 1.0),
        'c': nrm(ks[1], (BATCH, D), 1.0),
        'ctx': nrm(ks[2], (BATCH, CTX_LEN, D), 1.0),
        'c_ctx': nrm(ks[3], (D,), 1.0),
        'norm1_g': 1.0 + nrm(ks[4], (L, D), 0.02),
        'norm2_g': 1.0 + nrm(ks[5], (L, D), 0.02),
        'w_ada': nrm(ks[6], (L, D, 6 * D), 0.5 * D ** -0.5),
        'b_ada': nrm(ks[7], (L, 6 * D), 0.02),
        'w_in': nrm(ks[8], (L, D, PROJ_COLS), D ** -0.5),
        'conv_w': nrm(ks[9], (L, CONV_K, CONV_WIDTH), CONV_K ** -0.5),
        'w_out_conv': nrm(ks[10], (L, CONV_WIDTH, D), CONV_WIDTH ** -0.5),
        'lambda_q1': nrm(ks[11], (L, HEAD_DIM), 0.1),
        'lambda_k1': nrm(ks[12], (L, HEAD_DIM), 0.1),
        'lambda_q2': nrm(ks[13], (L, HEAD_DIM), 0.1),
        'lambda_k2': nrm(ks[14], (L, HEAD_DIM), 0.1),
        'subln_g': 1.0 + nrm(ks[15], (L, V_DIM), 0.02),
        'w_o_attn': nrm(ks[16], (L, ATTN_V, D), ATTN_V ** -0.5),
        'w_out': nrm(ks[17], (L, D, D), D ** -0.5),
        'w_router': nrm(ks[18], (L, D, E), D ** -0.5),
        'w_gate_e': nrm(ks[19], (L, E, D, F), D ** -0.5),
        'w_up_e': nrm(ks[20], (L, E, D, F), D ** -0.5),
        'w_down_e': nrm(ks[21], (L, E, F, D), F ** -0.5),
        'final_g': 1.0 + nrm(ks[22], (D,), 0.02),
    }


def reference(x, c, ctx, c_ctx, norm1_g, norm2_g, w_ada, b_ada, w_in, conv_w, w_out_conv,
              lambda_q1, lambda_k1, lambda_q2, lambda_k2, subln_g, w_o_attn, w_out,
              w_router, w_gate_e, w_up_e, w_down_e, final_g):
    n = x.shape[1]
    cos, sin = axial_rope_tables(n)
    f32 = jnp.float32
    for l in range(DEPTH):
        last = l == DEPTH - 1
        sh1, sc1, g1, sh2, sc2, g2 = ada_mod(c, w_ada[l], b_ada[l])
        sh1c, sc1c, g1c, sh2c, sc2c, g2c = ada_mod(c_ctx, w_ada[l], b_ada[l])
        lam_init = 0.8 - 0.6 * math.exp(-0.3 * l)
        lam = (jnp.exp(jnp.sum(lambda_q1[l].astype(f32) * lambda_k1[l].astype(f32)))
               - jnp.exp(jnp.sum(lambda_q2[l].astype(f32) * lambda_k2[l].astype(f32)))
               + lam_init)

        hx = modulate(rmsnorm(x, norm1_g[l]), sh1, sc1)
        hc = modulate(rmsnorm(ctx, norm1_g[l]), sh1c, sc1c)
        px = split_proj(hx @ w_in[l])
        pc = split_proj(hc @ w_in[l])
        qx, kx, vx = attn_heads(px)
        qc, kc, vc = attn_heads(pc)
        qx = apply_rope(qx, cos, sin)
        kx = apply_rope(kx, cos, sin)
        k_all = jnp.concatenate([to_bhm(kc), to_bhm(kx)], axis=3)
        v_all = jnp.concatenate([vc, vx], axis=2)
        ox = blocked_diff_attn(to_bhm(qx), k_all, v_all, lam)
        mix_x = merge_branches(px, ox, conv_w[l], w_out_conv[l], subln_g[l], lam_init,
                               w_o_attn[l], w_out[l])
        x = x + g1 * mix_x
        if not last:
            oc = diff_attn(to_bhm(qc), to_bhm(kc), vc, lam)
            ctx = ctx + g1c * merge_branches(pc, oc, conv_w[l], w_out_conv[l], subln_g[l],
                                             lam_init, w_o_attn[l], w_out[l])

        hx2 = modulate(rmsnorm(x, norm2_g[l]), sh2, sc2)
        x = x + g2 * expert_choice_ffn(hx2, w_router[l], w_gate_e[l], w_up_e[l], w_down_e[l])
        if not last:
            hc2 = modulate(rmsnorm(ctx, norm2_g[l]), sh2c, sc2c)
            ctx = ctx + g2c * expert_choice_ffn(hc2, w_router[l], w_gate_e[l], w_up_e[l], w_down_e[l])
    return rmsnorm(x, final_g)
```

```python
import numpy as np
from contextlib import ExitStack
import concourse.bass as bass
import concourse.mybir as mybir
from concourse.bass_utils import run_bass_kernel_spmd

F32 = mybir.dt.float32
BF16 = mybir.dt.bfloat16
U32 = mybir.dt.uint32
AF = mybir.ActivationFunctionType
ALU = mybir.AluOpType
AX = mybir.AxisListType

D = 1024
N = 2048
NCTX = 256
NK = N + NCTX
NT = N // 128
NKT = NK // 128
E = 16
CAP = 256
FH = 2048
EPS = 1e-6
EPOCH = 16000
NCONV = 10


class Sch:
    def __init__(self, nc, es, ndma=36):
        self.nc, self.es = nc, es
        self.eng = {"pe": nc.tensor, "act": nc.scalar, "dve": nc.vector, "pool": nc.gpsimd, "sp": nc.sync}
        self.sems = {e: [] for e in self.eng}
        self.cnt = {e: 0 for e in self.eng}
        self.obs = {e: {} for e in self.eng}
        self.last_w = {}
        self.readers = {}
        self.dsem = [es.enter_context(nc.semaphore(f"dq{i}")) for i in range(ndma)]
        self.dval = [0] * ndma
        third = ndma // 3
        self.dpool = {"sp": list(range(0, third)), "pool": list(range(third, 2 * third)),
                      "bg": list(range(2 * third, ndma))}
        self.dk = {"sp": 0, "pool": 0, "bg": 0}
        self.bg_prefix = ("cg", "cu", "cd")
        self.pe_pending = False

    def _csem(self, e, n):
        ep = (n - 1) // EPOCH
        while len(self.sems[e]) <= ep:
            self.sems[e].append(self.es.enter_context(self.nc.semaphore(f"s_{e}_{len(self.sems[e])}")))
        return self.sems[e][ep], (n - 1) % EPOCH + 1

    def _wait(self, e, key, n):
        if key[0] == "c":
            f = key[1]
            if f == e and e == "pe":
                return
            if self.obs[e].get(key, 0) >= n:
                return
            assert n <= self.cnt[f], f"wait on unsignalled {f} op ({n} > {self.cnt[f]})"
            sem, v = self._csem(f, n)
            self.eng[e].wait_ge(sem, v)
        else:
            if self.obs[e].get(key, 0) >= n:
                return
            self.eng[e].wait_ge(self.dsem[key[1]], n)
        self.obs[e][key] = n

    def op(self, e, fn, r=(), w=(), dma=False, sig=None, bg=False):
        w = list(w) + [b for b in r if isinstance(b, tuple) and b[0] == "ps"]
        need = {}

        def add(key, n):
            if need.get(key, 0) < n:
                need[key] = n

        for b in r:
            if b in self.last_w:
                add(*self.last_w[b])
        for b in w:
            if b in self.last_w:
                add(*self.last_w[b])
            for k, n in self.readers.get(b, {}).items():
                add(k, n)
        if dma:
            pk = "bg" if bg else e
            i = self.dpool[pk][self.dk[pk] % len(self.dpool[pk])]
            if self.dval[i] > 0:
                add(("d", i), self.dval[i])
        for k in sorted(need, key=str):
            self._wait(e, k, need[k])
        inst = fn(self.eng[e])
        if dma:
            self.dval[i] += 16
            inst.then_inc(self.dsem[i], 16)
            rec = (("d", i), self.dval[i])
            self.dk[pk] += 1
        elif e != "pe" or sig:
            self.cnt[e] += 1
            sem, v = self._csem(e, self.cnt[e])
            inst.then_inc(sem, 1)
            rec = (("c", e), self.cnt[e])
            if e == "pe":
                self.pe_pending = False
        else:
            rec = (("c", "pe"), self.cnt["pe"] + 1)
            self.pe_pending = True
        for b in r:
            d = self.readers.setdefault(b, {})
            if d.get(rec[0], 0) < rec[1]:
                d[rec[0]] = rec[1]
        for b in w:
            self.last_w[b] = rec
            self.readers[b] = {}
        return inst

    def barrier(self):
        assert not self.pe_pending
        for e in self.eng:
            for f in self.eng:
                if f != e and self.cnt[f] > 0:
                    self._wait(e, ("c", f), self.cnt[f])
            for i, v in enumerate(self.dval):
                if v > 0 and i not in self.dpool["bg"]:
                    self._wait(e, ("d", i), v)
        keep = {k: v for k, v in self.last_w.items() if isinstance(k, tuple) and k[0] in self.bg_prefix}
        self.last_w.clear()
        self.readers.clear()
        self.last_w.update(keep)

    def finish(self):
        for i, v in enumerate(self.dval):
            if v > 0:
                self._wait("sp", ("d", i), v)


def build_nc(stop_after=None, dbg=None):
    nc = bass.Bass("TRN2", target_bir_lowering=False)
    dt = nc.dram_tensor

    def din(name, shape):
        return dt(name, list(shape), F32, kind="ExternalInput").ap()

    x = din("x", [N, D])
    c = din("c", [1, D])
    ctx = din("ctx", [NCTX, D])
    c_ctx = din("c_ctx", [1, D])
    norm1_g = din("norm1_g", [1, D])
    norm2_g = din("norm2_g", [1, D])
    w_ada = din("w_ada", [D, 6 * D])
    b_ada = din("b_ada", [1, 6 * D])
    w_in = din("w_in", [D, 8 * D])
    conv_w = din("conv_w", [3, D])
    w_out_conv = din("w_out_conv", [D, D])
    lq1 = din("lambda_q1", [1, 64])
    lk1 = din("lambda_k1", [1, 64])
    lq2 = din("lambda_q2", [1, 64])
    lk2 = din("lambda_k2", [1, 64])
    subln_g = din("subln_g", [1, 128])
    w_o_attn = din("w_o_attn", [D, D])
    w_out = din("w_out", [D, D])
    w_router = din("w_router", [D, E])
    w_gate = din("w_gate_e", [E, D, FH])
    w_up = din("w_up_e", [E, D, FH])
    w_down = din("w_down_e", [E, FH, D])
    final_g = din("final_g", [1, D])
    cst = din("cst", [128, 2 * N + 128])
    out = dt("out", [N, D], F32, kind="ExternalOutput").ap()
    adas = dt("adas", [1, 8 * 512], F32, kind="Internal").ap()
    hx2d = dt("hx2d", [N, D], BF16, kind="Internal").ap()
    accd = dt("accd", [N, D], F32, kind="Internal").ap()
    wgb = dt("wgb", [NCONV, 8, 128, 2048], BF16, kind="Internal").ap()
    wub = dt("wub", [NCONV, 8, 128, 2048], BF16, kind="Internal").ap()
    wdb = dt("wdb", [NCONV, 8, 128, 2048], BF16, kind="Internal").ap()
    dbg_aps = {}
    if dbg:
        for name, shape in dbg.items():
            dbg_aps[name] = dt("dbg_" + name, list(shape), F32, kind="ExternalOutput").ap()

    es = ExitStack()
    with es:
        S = Sch(nc, es)

        def sb(name, shape, dtype, stack=es):
            return stack.enter_context(nc.sbuf_tensor(name, list(shape), dtype))

        ps = es.enter_context(nc.psum_tensor("ps", [128, 8, 512], F32))

        def psb(b):
            return ps[:, b, :].bitcast(BF16)

        def PB(*banks):
            return [("ps", b) for b in banks]

        identf = sb("identf", [128, 128], F32)
        ident = sb("ident", [128, 128], BF16)
        ones_bf = sb("ones_bf", [128, 128], BF16)
        ones_f = sb("ones_f", [128, 128], F32)
        U_bf = sb("U_bf", [128, 128], BF16)
        perm_bf = sb("perm_bf", [128, 128], BF16)
        iota_c = sb("iota_c", [128, CAP], F32)
        lamt = sb("lamt", [128, 8], F32)
        Lb = sb("Lb", [128, 16, 128], BF16)
        subg = sb("subg", [128, 128], F32)

        S.op("pool", lambda e: e.iota(identf[:], pattern=[[1, 128]], base=0, channel_multiplier=-1,
                                      allow_small_or_imprecise_dtypes=True), w=["identf"])
        S.op("dve", lambda e: e.tensor_single_scalar(out=U_bf[:], in_=identf[:], scalar=0.0, op=ALU.is_gt),
             r=["identf"], w=["U_bf"])
        S.op("dve", lambda e: e.tensor_single_scalar(out=ident[:], in_=identf[:], scalar=0.0, op=ALU.is_equal),
             r=["identf"], w=["ident"])
        S.op("dve", lambda e: e.tensor_single_scalar(out=identf[:], in_=identf[:], scalar=0.0, op=ALU.is_equal),
             r=["identf"], w=["identf"])
        S.op("dve", lambda e: e.memset(ones_bf[:], 1.0), w=["ones_bf"])
        S.op("dve", lambda e: e.memset(ones_f[:], 1.0), w=["ones_f"])
        S.op("pool", lambda e: e.iota(iota_c[:], pattern=[[1, CAP]], base=0, channel_multiplier=0,
                                      allow_small_or_imprecise_dtypes=True), w=["iota_c"])
        S.op("pool", lambda e: e.dma_start(out=perm_bf[:], in_=cst[:, 2 * N:2 * N + 128]), w=["perm_bf"], dma=True)

        with ExitStack() as es0:
            lt = sb("lt", [128, 4, 64], F32, es0)
            lj = sb("lj", [128, 64], F32, es0)
            for i, a in enumerate((lq1, lk1, lq2, lk2)):
                S.op("sp", lambda e, i=i, a=a: e.dma_start(out=lt[:, i, :], in_=a.partition_broadcast(128)),
                     w=[("lt", i)], dma=True)
            for i in range(2):
                S.op("dve", lambda e, i=i: e.tensor_tensor(out=lj[:], in0=lt[:, 2 * i, :], in1=lt[:, 2 * i + 1, :],
                                                           op=ALU.mult), r=[("lt", 2 * i), ("lt", 2 * i + 1)], w=["lj"])
                S.op("dve", lambda e, i=i: e.tensor_reduce(out=lamt[:, 2 + i:3 + i], in_=lj[:], axis=AX.X, op=ALU.add),
                     r=["lj"], w=["lam%d" % (2 + i)])
            S.op("act", lambda e: e.activation(out=lamt[:, 4:6], in_=lamt[:, 2:4], func=AF.Exp),
                 r=["lam2", "lam3"], w=["lam45"])
            S.op("dve", lambda e: e.tensor_tensor(out=lamt[:, 0:1], in0=lamt[:, 4:5], in1=lamt[:, 5:6],
                                                  op=ALU.subtract), r=["lam45"], w=["lam0"])
            S.op("dve", lambda e: e.tensor_scalar(out=lamt[:, 0:1], in0=lamt[:, 0:1], scalar1=0.2, scalar2=None,
                                                  op0=ALU.add), r=["lam0"], w=["lam0"])
            S.op("dve", lambda e: e.tensor_scalar(out=lamt[:, 1:2], in0=lamt[:, 0:1], scalar1=-1.0, scalar2=None,
                                                  op0=ALU.mult), r=["lam0"], w=["lam1"])
            S.op("sp", lambda e: e.dma_start(out=subg[:], in_=subln_g.partition_broadcast(128)), w=["subg"], dma=True)
            S.op("dve", lambda e: e.tensor_scalar(out=subg[:], in0=subg[:], scalar1=0.8, scalar2=None, op0=ALU.mult),
                 r=["subg"], w=["subg"])
            cc = sb("cc", [128, 16], F32, es0)
            S.op("sp", lambda e: e.dma_start(out=cc[:, 0:8], in_=c.rearrange("o (k p) -> p (o k)", p=128),
                                             allow_slow_non_contiguous=True), w=["cc0"], dma=True)
            S.op("sp", lambda e: e.dma_start(out=cc[:, 8:16], in_=c_ctx.rearrange("o (k p) -> p (o k)", p=128),
                                             allow_slow_non_contiguous=True), w=["cc1"], dma=True)
            S.op("act", lambda e: e.activation(out=cc[:], in_=cc[:], func=AF.Silu), r=["cc0", "cc1"],
                 w=["cc0", "cc1"])
            for k in range(16):
                S.op("dve", lambda e, k=k: e.tensor_scalar(out=Lb[:, k, :], in0=ones_f[:], scalar1=cc[:, k:k + 1],
                                                           scalar2=None, op0=ALU.mult),
                     r=["cc0", "cc1", "ones_f"], w=[("Lb", k)])
            S.barrier()

        wsl = [sb(f"wsl{i}", [128, 8, 512], BF16) for i in range(2)]
        wctr = [0]

        conv_per_load = [0]

        def load_w(src, ncols, nrow_chunks=8):
            if conv_per_load[0]:
                conv_emit(conv_per_load[0])
            s = wctr[0] % 2
            wctr[0] += 1
            v = src.rearrange("(k p) n -> p k n", p=128)
            half = nrow_chunks // 2
            for hh in range(2):
                S.op("pool", lambda e, hh=hh: e.dma_start(out=wsl[s][:, hh * half:(hh + 1) * half, 0:ncols],
                                                          in_=v[:, hh * half:(hh + 1) * half, :]),
                     w=[("wsl", s, hh)], dma=True)
            return s

        def WS(s):
            return [("wsl", s, 0), ("wsl", s, 1)]

        conv_chunks = []
        for e_ in range(NCONV):
            for fb in range(8):
                conv_chunks.append((wgb[e_, fb].rearrange("p (k n) -> p k n", k=8),
                                    w_gate[e_, :, fb * 256:(fb + 1) * 256].rearrange("(k p) n -> p k n", p=128), ("cg", e_, fb)))
                conv_chunks.append((wub[e_, fb].rearrange("p (k n) -> p k n", k=8),
                                    w_up[e_, :, fb * 256:(fb + 1) * 256].rearrange("(k p) n -> p k n", p=128), ("cu", e_, fb)))
                conv_chunks.append((wdb[e_, fb].rearrange("p (k n) -> p k n", k=2),
                                    w_down[e_, fb * 256:(fb + 1) * 256, :].rearrange("(k p) n -> p k n", p=128), ("cd", e_, fb)))
        conv_pos = [0]

        def conv_emit(n):
            for _ in range(n):
                if conv_pos[0] >= len(conv_chunks):
                    return
                dst, src, tok = conv_chunks[conv_pos[0]]
                conv_pos[0] += 1
                S.op("pool", lambda e: e.dma_start(out=dst, in_=src), w=[tok], dma=True, bg=True)

        def ada_load(g):
            return load_w(w_ada[:, g * 512:(g + 1) * 512], 512)

        def ada_group(g, lbase, dst, bbt, lbase2=None, dst2=None, s=None):
            if s is None:
                s = ada_load(g)
            S.op("sp", lambda e: e.dma_start(out=bbt[:], in_=b_ada[:, g * 512:(g + 1) * 512].partition_broadcast(128)),
                 w=["bbt"], dma=True)
            for lb, d_, bank in ((lbase, dst, g % 2), (lbase2, dst2, 2 + g % 2)):
                if lb is None:
                    continue
                for k in range(8):
                    S.op("pe", lambda e, k=k: e.matmul(ps[:, bank, :], lhsT=Lb[:, lb + k, :], rhs=wsl[s][:, k, :],
                                                       start=(k == 0), stop=(k == 7)),
                         r=WS(s) + [("Lb", lb + k)], w=PB(bank), sig=(k == 7))
                S.op("dve", lambda e: e.tensor_tensor(out=d_, in0=ps[:, bank, :], in1=bbt[:], op=ALU.add),
                     r=PB(bank) + ["bbt"], w=["adadst"])

        def dump(name, src_ap, row0, width, stack):
            if stack is es:
                dump.stg = xs2
            elif not hasattr(dump, "stg"):
                dump.stg = sb("dbgstg", [128, 1024], F32, stack)
            S.barrier()
            for c0 in range(0, width, 1024):
                cw_ = min(1024, width - c0)
                S.op("dve", lambda e: e.tensor_copy(out=dump.stg[:, 0:cw_], in_=src_ap[:, c0:c0 + cw_]), w=["dbgstg"])
                S.op("sp", lambda e: e.dma_start(out=dbg_aps[name][row0:row0 + 128, c0:c0 + cw_], in_=dump.stg[:, 0:cw_]),
                     r=["dbgstg"], w=[("dbgout", name, row0, c0)], dma=True)
            S.barrier()

        epsb = sb("epsb", [128, 1], F32)
        S.op("dve", lambda e: e.memset(epsb[:], EPS), w=["epsb"])
        ssq = sb("ssq", [128, 64], F32)
        rstd = sb("rstd", [128, 64], F32)
        R1 = sb("R1", [128, 8 * NK], BF16)

        def ma(k, t0, t1):
            return R1[:, k * N + t0:k * N + t1]

        def kTv(p0, p1, h, t0, t1):
            return R1[p0:p1, h * NK + t0:h * NK + t1]

        def norm_tile(src_ap, src_tok, A_bc, B_bc, hb, hb_tok, xs_t, sq, i):
            S.op("act", lambda e: e.activation(out=sq[:], in_=src_ap, func=AF.Square, accum_out=ssq[:, i:i + 1]),
                 r=src_tok, w=["sq", ("ssq", i)])
            S.op("act", lambda e: e.activation(out=rstd[:, i:i + 1], in_=ssq[:, i:i + 1], func=AF.Sqrt,
                                               bias=epsb[:, 0:1], scale=1.0 / D), r=[("ssq", i)], w=[("rstd", i)])
            S.op("dve", lambda e: e.reciprocal(out=rstd[:, i:i + 1], in_=rstd[:, i:i + 1]), r=[("rstd", i)],
                 w=[("rstd", i)])
            if B_bc is None:
                S.op("dve", lambda e: e.scalar_tensor_tensor(out=hb, in0=src_ap, scalar=rstd[:, i:i + 1], in1=A_bc,
                                                             op0=ALU.mult, op1=ALU.mult),
                     r=src_tok + [("rstd", i)], w=[hb_tok])
                return
            S.op("dve", lambda e: e.scalar_tensor_tensor(out=xs_t[:], in0=src_ap, scalar=rstd[:, i:i + 1], in1=A_bc,
                                                         op0=ALU.mult, op1=ALU.mult),
                 r=src_tok + [("rstd", i)], w=["xs_t"])
            S.op("dve", lambda e: e.tensor_tensor(out=hb, in0=xs_t[:], in1=B_bc, op=ALU.add),
                 r=["xs_t"], w=[hb_tok])

        es_mix = ExitStack()
        es_mix.__enter__()
        hT = sb("hT", [128, 8, NK], BF16, es_mix)
        oT = sb("oT", [128, 8, N], BF16, es_mix)

        with ExitStack() as es1:
            MB = sb("MB", [128, 2, D], F32, es1)
            MC = sb("MC", [128, 2, D], F32, es1)
            n1g = sb("n1g", [128, D], F32, es1)
            bbt = sb("bbt", [128, 512], F32, es1)
            xt = [sb(f"xt{i}", [128, D], F32, es1) for i in range(3)]
            xs_t = sb("xs_t", [128, D], F32, es1)
            sq = sb("sq", [128, D], BF16, es1)
            hb = [sb(f"hb{i}", [128, D], BF16, es1) for i in range(3)]
            S.op("sp", lambda e: e.dma_start(out=n1g[:], in_=norm1_g.partition_broadcast(128)), w=["n1g"], dma=True)
            for g in range(4):
                ada_group(g, 0, MB[:, g // 2, (g % 2) * 512:(g % 2 + 1) * 512], bbt,
                          lbase2=8, dst2=MC[:, g // 2, (g % 2) * 512:(g % 2 + 1) * 512])
            for M in (MB, MC):
                S.op("dve", lambda e, M=M: e.scalar_tensor_tensor(out=M[:, 1, :], in0=M[:, 1, :], scalar=1.0,
                                                                  in1=n1g[:], op0=ALU.add, op1=ALU.mult),
                     r=["adadst", "n1g"], w=["adadst"])
            S.barrier()
            def s1_A(j):
                s = j % 3
                src = x[j * 128:(j + 1) * 128, :] if j < NT else ctx[(j - NT) * 128:(j - NT + 1) * 128, :]
                M = MB if j < NT else MC
                S.op("sp", lambda e: e.dma_start(out=xt[s][:], in_=src), w=[("xt", s)], dma=True)
                norm_tile(xt[s][:], [("xt", s)], M[:, 1, :], M[:, 0, :], hb[s][:], ("hb", s), xs_t, sq, j)

            def s1_B(j):
                s = j % 3
                pb_ = j % 2
                for k in range(8):
                    S.op("pe", lambda e, k=k: e.transpose(out=psb(pb_)[:, k * 128:(k + 1) * 128],
                                                          in_=hb[s][:, k * 128:(k + 1) * 128], identity=ident[:]),
                         r=[("hb", s)], w=PB(pb_), sig=(k == 7))
                S.op("act", lambda e: e.activation(out=hT[:, :, j * 128:(j + 1) * 128],
                                                   in_=psb(pb_).rearrange("p (k t) -> p k t", k=8), func=AF.Copy),
                     r=PB(pb_), w=[("hT", j)])

            adatmp = sb("adatmp", [128, 512], F32, es1)
            ada_slots = {}
            ada_done = set()
            s1_A(0)
            s1_A(1)
            for j in range(NKT):
                if j + 2 < NKT:
                    s1_A(j + 2)
                s1_B(j)
                if j % 2 == 1 and j // 2 < 8:
                    ada_slots[j // 2] = ada_load(4 + j // 2)
                if j % 2 == 1 and j >= 3 and (j - 2) // 2 < 8 or j == NKT - 1:
                    for i in ([(j - 2) // 2] if j < NKT - 1 else [i_ for i_ in range(8) if i_ not in ada_done]):
                        if i in ada_done or i not in ada_slots:
                            continue
                        ada_done.add(i)
                        ada_group(4 + i, 0, adatmp[:], bbt, s=ada_slots[i])
                        S.op("sp", lambda e, i=i: e.dma_start(out=adas[0:1, i * 512:(i + 1) * 512], in_=adatmp[0:1, :]),
                             r=["adadst"], w=[("adas", i)], dma=True)
            if dbg and "hT" in dbg:
                for k in range(8):
                    dump("hT", hT[:, k, :], k * 128, NK, es1)
            S.barrier()

        def finish_early():
            es_mix.__exit__(None, None, None)
            S.finish()
            return nc

        if stop_after == 1:
            return finish_early()

        conv_per_load[0] = 0
        es_att = ExitStack()
        es_att.__enter__()
        vext = sb("vext", [128, NKT, 8, 130], BF16, es_att)
        rt1s = [sb(f"rt1_{i}", [128, 512], F32, es_att) for i in range(2)]
        rt2s = [sb(f"rt2_{i}", [128, 512], F32, es_att) for i in range(2)]
        kq0s = [sb(f"kq0_{i}", [128, 512], BF16, es_att) for i in range(2)]
        S.op("dve", lambda e: e.memset(vext[:, :, :, 128:130], 1.0), w=["vones"])
        bctr = [0]

        def rope_block(br, bs, C_ap, S_ap, dst_ap, dst_tok, extra_r):
            i = br % 2
            rt1, rt2, kq0 = rt1s[i], rt2s[i], kq0s[i]
            S.op("act", lambda e: e.activation(out=kq0[:], in_=ps[:, br, :], func=AF.Copy), r=PB(br), w=[("kq0", i)])
            S.op("pe", lambda e: e.matmul(ps[:, bs, :], lhsT=perm_bf[:], rhs=kq0[:], start=True, stop=True),
                 r=[("kq0", i)], w=PB(bs), sig=True)
            S.op("dve", lambda e: e.tensor_tensor(out=rt1[:], in0=ps[:, br, :], in1=C_ap, op=ALU.mult),
                 r=PB(br) + extra_r, w=[("rt1", i)])
            S.op("dve", lambda e: e.tensor_tensor(out=rt2[:], in0=ps[:, bs, :], in1=S_ap, op=ALU.mult),
                 r=PB(bs) + extra_r, w=[("rt2", i)])
            S.op("dve", lambda e: e.tensor_tensor(out=dst_ap, in0=rt1[:], in1=rt2[:], op=ALU.add),
                 r=[("rt1", i), ("rt2", i)], w=[dst_tok])

        def run_pipelined(blocks):
            if not blocks:
                return
            blocks[0][0]()
            for n, (mm_fn, post_fn) in enumerate(blocks):
                if n + 1 < len(blocks):
                    blocks[n + 1][0]()
                post_fn()

        with ExitStack() as es2:
            ropeC = sb("ropeC", [128, N], F32, es2)
            ropeS = sb("ropeS", [128, N], F32, es2)
            for q4 in range(4):
                S.op("sp", lambda e: e.dma_start(out=ropeC[:, q4 * 512:(q4 + 1) * 512], in_=cst[:, q4 * 512:(q4 + 1) * 512]),
                     w=[("ropeC", q4)], dma=True)
                S.op("sp", lambda e: e.dma_start(out=ropeS[:, q4 * 512:(q4 + 1) * 512],
                                                 in_=cst[:, N + q4 * 512:N + (q4 + 1) * 512]), w=[("ropeS", q4)], dma=True)
            for g in range(2):
                s = load_w(w_in[:, 4096 + g * 512:4096 + (g + 1) * 512], 512)
                blocks = []
                for hh in range(4):
                    for tb in range(5):
                        def mk(hh=hh, tb=tb, s=s, g=g):
                            h = g * 4 + hh
                            t0 = tb * 512
                            tw = 512 if tb < 4 else 256
                            br = bctr[0] % 2
                            bs = 2 + bctr[0] % 2
                            bctr[0] += 1

                            def mm():
                                for k in range(8):
                                    S.op("pe", lambda e, k=k: e.matmul(ps[:, br, 0:tw], lhsT=wsl[s][:, k, hh * 128:(hh + 1) * 128],
                                                                       rhs=hT[:, k, t0:t0 + tw], start=(k == 0), stop=(k == 7)),
                                         r=WS(s), w=PB(br), sig=(k == 7))

                            def post():
                                if tb < 4:
                                    rope_block(br, bs, ropeC[:, t0:t0 + 512], ropeS[:, t0:t0 + 512],
                                               kTv(0, 128, h, t0, t0 + 512), ("kT", h), [("ropeC", tb), ("ropeS", tb)])
                                else:
                                    S.op("act", lambda e: e.activation(out=kTv(0, 128, h, N, NK), in_=ps[:, br, 0:NCTX],
                                                                       func=AF.Copy), r=PB(br), w=[("kT", h)])
                            return mm, post
                        blocks.append(mk())
                run_pipelined(blocks)
            for g in range(2):
                s = load_w(w_in[:, 5120 + g * 512:5120 + (g + 1) * 512], 512)
                for j in range(NKT):
                    b = 4 + j % 2
                    for k in range(8):
                        S.op("pe", lambda e, k=k: e.matmul(ps[:, b, :], lhsT=hT[:, k, j * 128:(j + 1) * 128],
                                                           rhs=wsl[s][:, k, :], start=(k == 0), stop=(k == 7)),
                             r=WS(s), w=PB(b), sig=(k == 7))
                    S.op("act" if j % 2 else "dve",
                         lambda e: e.tensor_copy(out=vext[:, j, g * 4:(g + 1) * 4, 0:128],
                                                 in_=ps[:, b, :].rearrange("p (h v) -> p h v", h=4))
                         if j % 2 == 0 else
                         e.activation(out=vext[:, j, g * 4:(g + 1) * 4, 0:128],
                                      in_=ps[:, b, :].rearrange("p (h v) -> p h v", h=4), func=AF.Copy),
                         r=PB(b), w=["vext"])
            S.barrier()
        if stop_after == 1.5:
            for h in range(8):
                dump("kT", kTv(0, 128, h, 0, NK), h * 128, NK, es_att)
            for j in range(NKT):
                dump("v", vext[:, j, :, :].rearrange("p h v -> p (h v)"), j * 128, 1040, es_att)
            es_att.__exit__(None, None, None)
            return finish_early()

        qT = sb("qT", [128, 8, 512], BF16, es_att)
        ropeq = sb("ropeq", [128, 2, 512], F32, es_att)
        PT = [sb(f"PT{i}", [128, 2, 512], BF16, es_att) for i in range(3)]
        NPT = 3
        accs = sb("accs", [128, 8, 130], F32, es_att)
        rl = sb("rl", [128, 8, 1], F32, es_att)
        ow = sb("ow", [128, 4, 128], F32, es_att)
        ow2 = sb("ow2", [128, 4, 128], F32, es_att)
        sst = sb("sst", [128, 8], F32, es_att)
        ob = sb("ob", [128, 4, 128], BF16, es_att)
        pcnt = 0
        scnt = 0
        qslots = []
        for qb in range(4):
            q0 = qb * 512
            S.op("sp", lambda e: e.dma_start(out=ropeq[:, 0, :], in_=cst[:, q0:q0 + 512]), w=["ropeq0"], dma=True)
            S.op("sp", lambda e: e.dma_start(out=ropeq[:, 1, :], in_=cst[:, N + q0:N + q0 + 512]), w=["ropeq1"], dma=True)
            blocks = []
            for g in range(2):
                if qb == 0:
                    qslots.append(load_w(w_in[:, 3072 + g * 512:3072 + (g + 1) * 512], 512))
                for hh in range(4):
                    def mkq(g=g, hh=hh):
                        s = qslots[g]
                        h = g * 4 + hh
                        br = bctr[0] % 2
                        bs = 2 + bctr[0] % 2
                        bctr[0] += 1

                        def mm():
                            for k in range(8):
                                S.op("pe", lambda e, k=k: e.matmul(ps[:, br, :], lhsT=wsl[s][:, k, hh * 128:(hh + 1) * 128],
                                                                   rhs=hT[:, k, q0:q0 + 512], start=(k == 0), stop=(k == 7)),
                                     r=WS(s), w=PB(br), sig=(k == 7))

                        def post():
                            rope_block(br, bs, ropeq[:, 0, :], ropeq[:, 1, :], qT[:, h, :], ("qT", h), ["ropeq0", "ropeq1"])
                        return mm, post
                    blocks.append(mkq())
            run_pipelined(blocks)
            def emit_S(h, kc, b0):
                for m in range(2):
                    S.op("pe", lambda e, m=m: e.matmul(ps[:, b0 + m, :],
                                                       lhsT=kTv(m * 64, (m + 1) * 64, h, kc * 128, (kc + 1) * 128),
                                                       rhs=qT[m * 64:(m + 1) * 64, h, :], start=True, stop=True),
                         r=[("kT", h), ("qT", h)], w=PB(b0 + m), sig=(m == 1))

            def emit_exp(h, kc, b0):
                nonlocal pcnt
                pi = pcnt % 3
                pcnt += 1
                S.op("act", lambda e: e.activation(out=PT[pi][:], in_=ps[:, b0:b0 + 2, :], func=AF.Exp, scale=0.125),
                     r=PB(b0, b0 + 1), w=[("PT", pi)])
                return pi

            def emit_AV(h, kc, pi):
                for m in range(2):
                    for qt in range(4):
                        a = m * 4 + qt
                        bank = 4 + a // 2
                        off = (a % 2) * 256
                        last = (kc == NKT - 1)
                        S.op("pe", lambda e, m=m, qt=qt: e.matmul(
                            ps[:, bank, off:off + 129], lhsT=PT[pi][:, m, qt * 128:(qt + 1) * 128],
                            rhs=vext[:, kc, h, 0:129], start=(kc == 0 and a % 2 == 0), stop=last,
                            skip_group_check=True),
                            r=[("PT", pi), "vext", "vones"], w=PB(bank), sig=(last and a == 7))

            def epilogue(h):
                S.op("dve", lambda e: e.tensor_copy(out=accs[:, 0:4, 0:129], in_=ps[:, 4:6, :].rearrange("p b (a c) -> p (b a) c", a=2)[:, :, 0:129]),
                     r=PB(4, 5), w=["accs0"])
                S.op("dve", lambda e: e.tensor_copy(out=accs[:, 4:8, 0:129], in_=ps[:, 6:8, :].rearrange("p b (a c) -> p (b a) c", a=2)[:, :, 0:129]),
                     r=PB(6, 7), w=["accs1"])
                S.op("dve", lambda e: e.reciprocal(out=rl[:], in_=accs[:, :, 128:129]), r=["accs0", "accs1"], w=["rl"])
                S.op("dve", lambda e: e.tensor_scalar(out=rl[:, 4:8, :], in0=rl[:, 4:8, :], scalar1=lamt[:, 1:2],
                                                      scalar2=None, op0=ALU.mult), r=["rl"], w=["rl"])
                S.op("dve", lambda e: e.tensor_tensor(out=ow[:], in0=accs[:, 0:4, 0:128],
                                                      in1=rl[:, 0:4, :].broadcast_to([128, 4, 128]), op=ALU.mult),
                     r=["rl", "accs0"], w=["ow"])
                S.op("dve", lambda e: e.tensor_tensor(out=ow2[:], in0=accs[:, 4:8, 0:128],
                                                      in1=rl[:, 4:8, :].broadcast_to([128, 4, 128]), op=ALU.mult),
                     r=["rl", "accs1"], w=["ow2"])
                S.op("dve", lambda e: e.tensor_tensor(out=ow[:], in0=ow[:], in1=ow2[:], op=ALU.add),
                     r=["ow", "ow2"], w=["ow"])
                S.op("dve", lambda e: e.tensor_tensor(out=ow2[:], in0=ow[:], in1=ow[:], op=ALU.mult),
                     r=["ow"], w=["ow2"])
                S.op("dve", lambda e: e.tensor_reduce(out=sst[:, 0:4], in_=ow2[:], axis=AX.X, op=ALU.add),
                     r=["ow2"], w=["sst"])
                def part_b():
                    S.op("act", lambda e: e.activation(out=sst[:, 4:8], in_=sst[:, 0:4], func=AF.Ln, bias=epsb[:, 0:1],
                                                       scale=1.0 / 128), r=["sst"], w=["sst2"])
                    S.op("act", lambda e: e.activation(out=sst[:, 4:8], in_=sst[:, 4:8], func=AF.Exp, scale=-0.5),
                         r=["sst2"], w=["sst2"])
                    S.op("dve", lambda e: e.tensor_tensor(out=ow[:], in0=ow[:],
                                                          in1=sst[:, 4:8].unsqueeze(2).broadcast_to([128, 4, 128]),
                                                          op=ALU.mult), r=["ow", "sst2"], w=["ow"])
                    S.op("dve", lambda e: e.tensor_tensor(out=ob[:], in0=ow[:],
                                                          in1=subg[:].unsqueeze(1).broadcast_to([128, 4, 128]),
                                                          op=ALU.mult), r=["ow"], w=["ob"])

                def do_transposes(bt):
                    for qt in range(4):
                        S.op("pe", lambda e, qt=qt: e.transpose(out=psb(bt)[:, qt * 128:(qt + 1) * 128], in_=ob[:, qt, :],
                                                                identity=ident[:]), r=["ob"], w=PB(bt), sig=(qt == 3))
                    S.op("dve", lambda e: e.tensor_copy(out=oT[:, h, q0:q0 + 512], in_=psb(bt)[:, 0:512]),
                         r=PB(bt), w=["oT"])

                return part_b, do_transposes

            items = [(h, kc) for h in range(8) for kc in range(NKT)]
            nit = len(items)
            emit_S(*items[0], 0)
            emit_S(*items[1], 2)
            pending_tr = None
            for idx, (h, kc) in enumerate(items):
                b0 = (idx % 2) * 2
                gidx = qb * nit + idx + 1
                tgt = (gidx * len(conv_chunks)) // (4 * nit)
                if tgt > conv_pos[0]:
                    conv_emit(tgt - conv_pos[0])
                pi = emit_exp(h, kc, b0)
                if kc == 9 and pending_tr is not None:
                    pending_tr[0]()
                if kc == 12 and pending_tr is not None:
                    pending_tr[1](b0)
                    pending_tr = None
                    emit_AV(h, kc, pi)
                    if idx + 2 < nit:
                        emit_S(*items[idx + 2], b0)
                else:
                    if idx + 2 < nit:
                        emit_S(*items[idx + 2], b0)
                    emit_AV(h, kc, pi)
                if kc == NKT - 1:
                    pending_tr = epilogue(h)
            pending_tr[0]()
            pending_tr[1](0)
        if dbg and "oT" in dbg:
            for k in range(8):
                dump("oT", oT[:, k, :], k * 128, N, es_att)
        S.barrier()
        es_att.__exit__(None, None, None)
        if stop_after == 2:
            return finish_early()

        def gated_proj(w_main, src_fn, gate_col0, accumulate, tmp):
            sgt, tt = tmp
            for hg in range(4):
                s1 = wctr[0] % 2
                wctr[0] += 1
                S.op("pool", lambda e: e.dma_start(out=wsl[s1][:, :, 0:256],
                                                   in_=w_main[:, hg * 256:(hg + 1) * 256].rearrange("(k p) n -> p k n", p=128)),
                     w=[("wsl", s1, 0)], dma=True)
                S.op("pool", lambda e: e.dma_start(out=wsl[s1][:, :, 256:512],
                                                   in_=w_in[:, gate_col0 + hg * 256:gate_col0 + (hg + 1) * 256].rearrange("(k p) n -> p k n", p=128)),
                     w=[("wsl", s1, 1)], dma=True)
                for dl in range(2):
                    dg = hg * 2 + dl
                    for tb in range(4):
                        t0 = tb * 512
                        ba = bctr[0] % 2
                        bg = 2 + bctr[0] % 2
                        bctr[0] += 1
                        for k in range(8):
                            S.op("pe", lambda e, k=k: e.matmul(ps[:, ba, :], lhsT=wsl[s1][:, k, dl * 128:(dl + 1) * 128],
                                                               rhs=src_fn(k, t0), start=(k == 0), stop=(k == 7)),
                                 r=[("wsl", s1, 0), "gsrc"], w=PB(ba), sig=(k == 7))
                        for k in range(8):
                            S.op("pe", lambda e, k=k: e.matmul(ps[:, bg, :], lhsT=wsl[s1][:, k, 256 + dl * 128:256 + (dl + 1) * 128],
                                                               rhs=hT[:, k, t0:t0 + 512], start=(k == 0), stop=(k == 7)),
                                 r=[("wsl", s1, 1)], w=PB(bg), sig=(k == 7))
                        S.op("act", lambda e: e.activation(out=sgt[:], in_=ps[:, bg, :], func=AF.Sigmoid),
                             r=PB(bg), w=["sgt"])
                        if not accumulate:
                            S.op("dve", lambda e: e.tensor_tensor(out=ma(dg, t0, t0 + 512), in0=ps[:, ba, :], in1=sgt[:],
                                                                  op=ALU.mult), r=PB(ba) + ["sgt"], w=[("ma", dg)])
                        else:
                            S.op("dve", lambda e: e.tensor_tensor(out=tt[:], in0=ps[:, ba, :], in1=sgt[:], op=ALU.mult),
                                 r=PB(ba) + ["sgt"], w=["tt"])
                            S.op("dve", lambda e: e.tensor_tensor(out=ma(dg, t0, t0 + 512), in0=tt[:],
                                                                  in1=ma(dg, t0, t0 + 512), op=ALU.add),
                                 r=["tt", ("ma", dg)], w=[("ma", dg)])

        es_de = ExitStack()
        es_de.__enter__()
        sgt = sb("sgt", [128, 512], F32, es_de)
        tt = sb("tt", [128, 512], F32, es_de)
        gated_proj(w_o_attn, lambda k, t0: oT[:, k, t0:t0 + 512], 7168, False, (sgt, tt))

        yc = sb("yc", [128, 8, N], BF16, es_de)
        cw = sb("cw", [128, 8, 3], F32, es_de)
        zt = [sb(f"zt{i}", [128, N + 2], F32, es_de) for i in range(2)]
        usb = sb("usb", [128, 512], F32, es_de)
        gbs = sb("gbs", [128, N], BF16, es_de)
        cacc = sb("cacc", [128, N], F32, es_de)
        for kk in range(3):
            S.op("sp", lambda e, kk=kk: e.dma_start(out=cw[:, :, kk], in_=conv_w[kk:kk + 1, :].rearrange("o (j p) -> p (o j)", p=128),
                                                    allow_slow_non_contiguous=True), w=[("cw", kk)], dma=True)
        for i in range(2):
            S.op("dve", lambda e, i=i: e.memset(zt[i][:, 0:1], 0.0), w=[("ztp", i)])
            S.op("dve", lambda e, i=i: e.memset(zt[i][:, N + 1:N + 2], 0.0), w=[("ztp", i)])
        for j in range(8):
            zi = j % 2
            s = wctr[0] % 2
            wctr[0] += 1
            for part in range(3):
                S.op("pool", lambda e, part=part: e.dma_start(
                    out=wsl[s][:, :, part * 128:(part + 1) * 128],
                    in_=w_in[:, part * 1024 + j * 128:part * 1024 + (j + 1) * 128].rearrange("(k p) n -> p k n", p=128)),
                    w=[("wsl", s, 0), ("wsl", s, 1)], dma=True)
            for tb in range(4):
                t0 = tb * 512
                banks = [(bctr[0] % 2) * 3 + i for i in range(3)]
                bctr[0] += 1
                for part in range(3):
                    for k in range(8):
                        S.op("pe", lambda e, k=k, part=part: e.matmul(
                            ps[:, banks[part], :], lhsT=wsl[s][:, k, part * 128:(part + 1) * 128],
                            rhs=hT[:, k, t0:t0 + 512], start=(k == 0), stop=(k == 7)),
                            r=WS(s), w=PB(banks[part]), sig=(k == 7))
                S.op("act", lambda e: e.activation(out=usb[:], in_=ps[:, banks[0], :], func=AF.Copy),
                     r=PB(banks[0]), w=["usb"])
                S.op("dve", lambda e: e.tensor_tensor(out=zt[zi][:, 1 + t0:1 + t0 + 512], in0=ps[:, banks[2], :],
                                                      in1=usb[:], op=ALU.mult), r=PB(banks[2]) + ["usb"], w=[("zt", zi)])
                S.op("act", lambda e: e.activation(out=gbs[:, t0:t0 + 512], in_=ps[:, banks[1], :], func=AF.Copy),
                     r=PB(banks[1]), w=["gbs"])
            zr = [("zt", zi), ("ztp", zi), ("cw", 0), ("cw", 1), ("cw", 2)]
            S.op("dve", lambda e: e.tensor_scalar(out=cacc[:], in0=zt[zi][:, 0:N], scalar1=cw[:, j, 0:1], scalar2=None,
                                                  op0=ALU.mult), r=zr, w=["cacc"])
            S.op("dve", lambda e: e.scalar_tensor_tensor(out=cacc[:], in0=zt[zi][:, 1:N + 1], scalar=cw[:, j, 1:2],
                                                         in1=cacc[:], op0=ALU.mult, op1=ALU.add), r=zr + ["cacc"], w=["cacc"])
            S.op("dve", lambda e: e.scalar_tensor_tensor(out=cacc[:], in0=zt[zi][:, 2:N + 2], scalar=cw[:, j, 2:3],
                                                         in1=cacc[:], op0=ALU.mult, op1=ALU.add), r=zr + ["cacc"], w=["cacc"])
            S.op("dve", lambda e: e.tensor_tensor(out=yc[:, j, :], in0=cacc[:], in1=gbs[:], op=ALU.mult),
                 r=["cacc", "gbs"], w=["gsrc"])
        gated_proj(w_out_conv, lambda k, t0: yc[:, k, t0:t0 + 512], 6144, True, (sgt, tt))
        if dbg and "merged" in dbg:
            for k in range(8):
                dump("merged", ma(k, 0, N), k * 128, N, es_de)
        S.barrier()
        es_de.__exit__(None, None, None)
        es_mix.__exit__(None, None, None)
        if stop_after == 3:
            S.finish()
            return nc

        X = sb("X", [128, NT, D], F32)
        bbt = sb("bbt2", [128, 512], F32)
        tf = sb("tf", [128, 512], F32)
        g2fg = sb("g2fg", [128, 2, D], F32)
        AFF = sb("AFF", [128, NT, E], F32)
        AFHL = sb("AFHL", [128, NT, E, 4], BF16)
        MASKb = sb("MASKb", [128, NT, E], BF16)
        MASKf = sb("MASKf", [128, NT, E], F32)
        POSM = sb("POSM", [128, NT, E], F32)
        rsm = sb("rsm", [128, 3, NT], F32)
        dstg = [tf]

        def dump2(name, src_ap, row0, width):
            stg = dstg[0]
            cwid = stg.shape[-1]
            S.barrier()
            for c0 in range(0, width, cwid):
                cw_ = min(cwid, width - c0)
                S.op("dve", lambda e: e.tensor_copy(out=stg[:, 0:cw_], in_=src_ap[:, c0:c0 + cw_]), w=["dbgstg"])
                S.op("sp", lambda e: e.dma_start(out=dbg_aps[name][row0:row0 + 128, c0:c0 + cw_], in_=stg[:, 0:cw_]),
                     r=["dbgstg"], w=[("dbgout", name, row0, c0)], dma=True)
            S.barrier()

        with ExitStack() as esf:
            g1t = sb("g1t", [128, D], F32, esf)
            S.op("sp", lambda e: e.dma_start(out=g1t[:], in_=adas[0:1, 0:D].partition_broadcast(128)), w=["g1t"], dma=True)
            S.barrier()
            sw = [load_w(w_out[:, g * 512:(g + 1) * 512], 512) for g in range(2)]
            for j in range(NT):
                S.op("sp", lambda e: e.dma_start(out=X[:, j, :], in_=x[j * 128:(j + 1) * 128, :]), w=[("X", j)], dma=True)
                for g in range(2):
                    b = bctr[0] % 4
                    bctr[0] += 1
                    for k in range(8):
                        S.op("pe", lambda e, k=k: e.matmul(ps[:, b, :], lhsT=ma(k, j * 128, (j + 1) * 128),
                                                           rhs=wsl[sw[g]][:, k, :], start=(k == 0), stop=(k == 7)),
                             r=WS(sw[g]), w=PB(b), sig=(k == 7))
                    S.op("dve", lambda e: e.tensor_tensor(out=tf[:], in0=ps[:, b, :], in1=g1t[:, g * 512:(g + 1) * 512],
                                                          op=ALU.mult), r=PB(b), w=["tf"])
                    S.op("dve", lambda e: e.tensor_tensor(out=X[:, j, g * 512:(g + 1) * 512], in0=tf[:],
                                                          in1=X[:, j, g * 512:(g + 1) * 512], op=ALU.add),
                         r=["tf", ("X", j)], w=[("X", j)])
            if dbg and "x1" in dbg:
                for j in range(NT):
                    dump2("x1", X[:, j, :], j * 128, D)
            S.barrier()
        if stop_after == 4:
            S.finish()
            return nc

        def HX2(j, d0=0, d1=D):
            return R1[:, j * D + d0:j * D + d1]

        conv_emit(len(conv_chunks))
        with ExitStack() as esg:
            ab2 = sb("ab2", [128, 2, D], F32, esg)
            xs2 = sb("xs2", [128, D], F32, esg)
            sq2 = sb("sq2", [128, D], BF16, esg)
            wr = sb("wr", [128, 8, E], BF16, esg)
            hx2T = [sb(f"hx2T{i}", [128, 8, 128], BF16, esg) for i in range(2)]
            zt0 = sb("zeros0", [128, D], F32, esg)
            S.op("dve", lambda e: e.memset(zt0[:], 0.0), w=["zt0"])
            S.op("sp", lambda e: e.dma_start(out=ab2[:, 1, :], in_=adas[0:1, D:2 * D].partition_broadcast(128)),
                 w=["adadst"], dma=True)
            S.op("sp", lambda e: e.dma_start(out=ab2[:, 0, :], in_=adas[0:1, 2 * D:3 * D].partition_broadcast(128)),
                 w=["adadst"], dma=True)
            S.op("sp", lambda e: e.dma_start(out=g2fg[:, 0, :], in_=adas[0:1, 3 * D:4 * D].partition_broadcast(128)),
                 w=["g2row"], dma=True)
            S.op("sp", lambda e: e.dma_start(out=xs2[:], in_=norm2_g.partition_broadcast(128)), w=["xs2"], dma=True)
            S.op("dve", lambda e: e.scalar_tensor_tensor(out=ab2[:, 0, :], in0=ab2[:, 0, :], scalar=1.0, in1=xs2[:],
                                                         op0=ALU.add, op1=ALU.mult), r=["adadst", "xs2"], w=["adadst"])
            S.op("sp", lambda e: e.dma_start(out=g2fg[:, 1, :], in_=final_g.partition_broadcast(128)), w=["fg"], dma=True)
            S.op("pool", lambda e: e.dma_start(out=wr[:], in_=w_router.rearrange("(k p) n -> p k n", p=128)), w=["wr"], dma=True)
            S.barrier()
            def g_A(j):
                norm_tile(X[:, j, :], [("X", j)], ab2[:, 0, :], ab2[:, 1, :], HX2(j), ("HX2", j), xs2, sq2, 20 + j)
                S.op("sp", lambda e: e.dma_start(out=hx2d[j * 128:(j + 1) * 128, :], in_=HX2(j)), r=[("HX2", j)],
                     w=[("hx2d", j)], dma=True)
                S.op("sp", lambda e: e.dma_start(out=accd[j * 128:(j + 1) * 128, :], in_=zt0[:]), r=["zt0"],
                     w=[("accd", j)], dma=True)

            def g_B(j):
                s = j % 2
                for k in range(8):
                    S.op("pe", lambda e, k=k: e.transpose(out=psb(s)[:, k * 128:(k + 1) * 128], in_=HX2(j, k * 128, (k + 1) * 128),
                                                          identity=ident[:]), r=[("HX2", j)], w=PB(s), sig=(k == 7))
                S.op("act", lambda e: e.activation(out=hx2T[s][:], in_=psb(s).rearrange("p (k t) -> p k t", k=8), func=AF.Copy),
                     r=PB(s), w=[("hx2T", s)])
                b2 = 2 + s
                for k in range(8):
                    S.op("pe", lambda e, k=k: e.matmul(ps[:, b2, 0:E], lhsT=hx2T[s][:, k, :], rhs=wr[:, k, :],
                                                       start=(k == 0), stop=(k == 7)), r=[("hx2T", s), "wr"], w=PB(b2), sig=(k == 7))

            def g_C(j):
                b2 = 2 + j % 2
                S.op("dve", lambda e: e.tensor_reduce(out=rsm[:, 0, j:j + 1], in_=ps[:, b2, 0:E], axis=AX.X, op=ALU.max),
                     r=PB(b2), w=[("mx", j)])
                S.op("dve", lambda e: e.tensor_scalar(out=rsm[:, 0, j:j + 1], in0=rsm[:, 0, j:j + 1], scalar1=-1.0, scalar2=None,
                                                      op0=ALU.mult), r=[("mx", j)], w=[("mx", j)])
                S.op("act", lambda e: e.activation(out=AFF[:, j, :], in_=ps[:, b2, 0:E], func=AF.Exp, bias=rsm[:, 0, j:j + 1],
                                                   scale=1.0, accum_out=rsm[:, 1, j:j + 1]), r=PB(b2) + [("mx", j)],
                     w=[("AFF", j), ("se", j)])
                S.op("dve", lambda e: e.reciprocal(out=rsm[:, 2, j:j + 1], in_=rsm[:, 1, j:j + 1]), r=[("se", j)], w=[("rs", j)])
                S.op("dve", lambda e: e.tensor_scalar(out=AFF[:, j, :], in0=AFF[:, j, :], scalar1=rsm[:, 2, j:j + 1], scalar2=None,
                                                      op0=ALU.mult), r=[("AFF", j), ("rs", j)], w=[("AFF", j)])

            g_A(0)
            g_A(1)
            for j in range(NT):
                if j + 2 < NT:
                    g_A(j + 2)
                g_B(j)
                if j >= 1:
                    g_C(j - 1)
            g_C(NT - 1)
            S.barrier()

        with ExitStack() as esh:
            tmpf = sb("tmpf", [128, NT * E], F32, esh)
            mid = sb("mid", [128, E], F32, esh)
            cnt = sb("cnt", [128, E], F32, esh)
            gew = sb("gew", [128, E], F32, esh)
            mk = sb("mk", [128, NT, E], BF16, esh)
            NIT = 22
            S.op("dve", lambda e: e.memset(mid[:], 0.5), w=["mid"])
            for it in range(NIT + 1):
                w_i = 2.0 ** -(it + 1)
                last = (it == NIT)
                if last:
                    S.op("dve", lambda e: e.tensor_scalar(out=mid[:], in0=mid[:], scalar1=-w_i, scalar2=None, op0=ALU.add),
                         r=["mid"], w=["mid"])
                S.op("dve", lambda e: e.tensor_tensor(out=(MASKb if last else mk)[:], in0=AFF[:],
                                                      in1=mid[:].unsqueeze(1).broadcast_to([128, NT, E]), op=ALU.is_ge),
                     r=["mid"], w=["MASKb" if last else "mk"])
                if last:
                    break
                bk_ = it % 2
                S.op("pe", lambda e: e.matmul(ps[:, bk_, 0:NT * E], lhsT=ones_bf[:], rhs=mk[:].rearrange("p j e -> p (j e)"),
                                              start=True, stop=True), r=["mk"], w=PB(bk_), sig=True)
                S.op("dve", lambda e: e.tensor_reduce(out=cnt[:], in_=ps[:, bk_, 0:NT * E].rearrange("p (j e) -> p e j", e=E),
                                                      axis=AX.X, op=ALU.add), r=PB(bk_), w=["cnt"])
                S.op("dve", lambda e: e.tensor_scalar(out=gew[:], in0=cnt[:], scalar1=CAP - 0.5, scalar2=w_i, op0=ALU.is_ge,
                                                      op1=ALU.mult), r=["cnt"], w=["gew"])
                S.op("dve", lambda e: e.scalar_tensor_tensor(out=mid[:], in0=gew[:], scalar=-w_i / 2, in1=mid[:], op0=ALU.add,
                                                             op1=ALU.add), r=["gew", "mid"], w=["mid"])
            S.op("dve", lambda e: e.tensor_copy(out=MASKf[:], in_=MASKb[:]), r=["MASKb"], w=["MASKf"])
            for j in range(NT):
                for jp in range(j):
                    S.op("pe", lambda e, jp=jp: e.matmul(ps[:, 5, j * E:(j + 1) * E], lhsT=ones_bf[:], rhs=MASKb[:, jp, :],
                                                         start=(jp == 0), stop=False), r=["MASKb"], w=PB(5))
                S.op("pe", lambda e: e.matmul(ps[:, 5, j * E:(j + 1) * E], lhsT=U_bf[:], rhs=MASKb[:, j, :],
                                              start=(j == 0), stop=True), r=["MASKb"], w=PB(5), sig=(j == NT - 1))
            S.op("dve", lambda e: e.scalar_tensor_tensor(out=POSM[:].rearrange("p j e -> p (j e)"), in0=ps[:, 5, 0:NT * E],
                                                         scalar=1.0, in1=MASKf[:].rearrange("p j e -> p (j e)"),
                                                         op0=ALU.add, op1=ALU.mult), r=PB(5) + ["MASKf"], w=["POSM"])
            S.op("dve", lambda e: e.tensor_scalar(out=POSM[:], in0=POSM[:], scalar1=-1.0, scalar2=None, op0=ALU.add),
                 r=["POSM"], w=["POSM"])
            S.op("dve", lambda e: e.tensor_copy(out=AFHL[:, :, :, 0], in_=AFF[:]), w=["AFH"])
            S.op("dve", lambda e: e.tensor_tensor(out=tmpf[:].rearrange("p (j e) -> p j e", e=E), in0=AFF[:],
                                                  in1=AFHL[:, :, :, 0], op=ALU.subtract), r=["AFH"], w=["tmpf"])
            S.op("dve", lambda e: e.tensor_copy(out=AFHL[:, :, :, 1], in_=tmpf[:].rearrange("p (j e) -> p j e", e=E)),
                 r=["tmpf"], w=["AFL"])
            pf = sb("pf", [128, NT], F32, esh)
            jf = sb("jf", [128, NT], F32, esh)
            S.op("pool", lambda e: e.iota(pf[:], pattern=[[0, NT]], base=0, channel_multiplier=1,
                                          allow_small_or_imprecise_dtypes=True), w=["pj0"])
            S.op("pool", lambda e: e.iota(jf[:], pattern=[[1, NT]], base=0, channel_multiplier=0,
                                          allow_small_or_imprecise_dtypes=True), w=["pj1"])
            S.op("dve", lambda e: e.tensor_copy(out=AFHL[:, :, :, 2], in_=pf[:].unsqueeze(2).broadcast_to([128, NT, E])),
                 r=["pj0"], w=["AFPJ"])
            S.op("dve", lambda e: e.tensor_copy(out=AFHL[:, :, :, 3], in_=jf[:].unsqueeze(2).broadcast_to([128, NT, E])),
                 r=["pj1", "AFPJ"], w=["AFPJ"])
            if dbg and "aff" in dbg:
                dump2("aff", AFF[:].rearrange("p j e -> p (j e)"), 0, NT * E)
                dump2("posm", POSM[:].rearrange("p j e -> p (j e)"), 0, NT * E)
            S.barrier()

        with ExitStack() as esi:
            Ssl = [sb(f"Ssl{i}", [128, NT, CAP], BF16, esi) for i in range(2)]
            xtok = [sb("xtok0", [128, 2, D], BF16, esi)] * 2
            xsTs = [sb(f"xsT{i}", [128, 8, CAP], BF16, esi) for i in range(2)]
            h1T = sb("h1T", [128, 16, CAP], BF16, esi)
            ysls = [sb("ysl0", [128, 2, D], BF16, esi)] * 2
            gsl = sb("gsl", [128, 2, 2], F32, esi)
            t8 = sb("t8", [128, 2, 4], F32, esi)
            idxf = sb("idxf", [128, 2], F32, esi)
            IDX = [[sb(f"IDX{i}_{c}", [128, 1], U32, esi) for c in range(2)] for i in range(3)]
            wdl = [sb(f"wdl{i}", [128, 2, D], BF16, esi) for i in range(3)]
            wgl = [wsl[0], wsl[1], sb("wsl2", [128, 8, 512], BF16, esi)]
            sa = tf
            fcnt = [0]

            def build_S(e_):
                si = e_ % 2
                for j in range(NT):
                    S.op("dve", lambda e, j=j: e.tensor_scalar(out=Ssl[si][:, j, :], in0=iota_c[:],
                                                               scalar1=POSM[:, j, e_:e_ + 1], scalar2=None,
                                                               op0=ALU.is_equal), w=[("S", si)])

            def slot_index(e_):
                si = e_ % 2
                for cc in range(2):
                    for j in range(NT):
                        S.op("pe", lambda e, j=j: e.matmul(ps[:, 6, cc * 4:cc * 4 + 4], lhsT=Ssl[si][:, j, cc * 128:(cc + 1) * 128],
                                                           rhs=AFHL[:, j, e_, :], start=(j == 0), stop=(j == NT - 1)),
                             r=[("S", si), "AFH", "AFL", "AFPJ"], w=PB(6), sig=(j == NT - 1 and cc == 1))
                S.op("dve", lambda e: e.tensor_copy(out=t8[:].rearrange("p c f -> p (c f)"), in_=ps[:, 6, 0:8]), r=PB(6), w=["t8"])
                S.op("dve", lambda e: e.tensor_tensor(out=gsl[:, si, :], in0=t8[:, :, 0], in1=t8[:, :, 1], op=ALU.add),
                     r=["t8"], w=[("gsl", si)])
                S.op("dve", lambda e: e.scalar_tensor_tensor(out=idxf[:], in0=t8[:, :, 3], scalar=128.0, in1=t8[:, :, 2],
                                                             op0=ALU.mult, op1=ALU.add), r=["t8"], w=["idxf"])
                for cc in range(2):
                    S.op("dve", lambda e, cc=cc: e.tensor_copy(out=IDX[e_ % 3][cc][:], in_=idxf[:, cc:cc + 1]), r=["idxf"],
                         w=[("IDX", e_ % 3, cc)])

            def gather(e_):
                si = e_ % 2
                for cc in range(2):
                    S.op("pool", lambda e, cc=cc: e.indirect_dma_start(
                        out=xtok[si][:, cc, :], out_offset=None, in_=hx2d[:, :],
                        in_offset=bass.IndirectOffsetOnAxis(IDX[e_ % 3][cc][:, 0:1], 0)),
                        r=[("IDX", e_ % 3, cc)], w=[("xtok", cc)], dma=True)

            def transposes(e_):
                si = e_ % 2
                for hf in range(2):
                    bank = 6 + hf
                    for kk in range(4):
                        kd = hf * 4 + kk
                        for cc in range(2):
                            S.op("pe", lambda e, cc=cc: e.transpose(
                                out=psb(bank)[:, kk * CAP + cc * 128:kk * CAP + (cc + 1) * 128],
                                in_=xtok[si][:, cc, kd * 128:(kd + 1) * 128], identity=ident[:]),
                                r=[("xtok", cc)], w=PB(bank), sig=(kk == 3 and cc == 1))
                    S.op("act", lambda e: e.activation(out=xsTs[si][:, hf * 4:(hf + 1) * 4, :],
                                                       in_=psb(bank).rearrange("p (k c) -> p k c", k=4), func=AF.Copy),
                         r=PB(bank), w=[("xsT", si)])

            def scatter(e_):
                si = e_ % 2
                for cc in range(2):
                    S.op("pool", lambda e, cc=cc: e.indirect_dma_start(
                        out=accd[:, :], out_offset=bass.IndirectOffsetOnAxis(IDX[e_ % 3][cc][:, 0:1], 0),
                        in_=ysls[si][:, cc, :], in_offset=None, compute_op=ALU.add),
                        r=[("IDX", e_ % 3, cc), ("ysl", cc)], w=["accd_acc"], dma=True)

            build_S(0)
            slot_index(0)
            if dbg and "idx0" in dbg:
                dump2("idx0", idxf[:], 0, 2)
                dump2("idx0", gsl[:, 0, :], 128, 2)
            gather(0)
            build_S(1)
            transposes(0)
            for e_ in range(E):
                si = e_ % 2
                xsT = xsTs[si]
                pend = None
                for fb in range(8):
                    s = fcnt[0] % 3
                    fcnt[0] += 1
                    conv = e_ < NCONV
                    wflat = wgl[s][:].rearrange("p k n -> p (k n)")
                    gdst = wflat[:, 0:2048].rearrange("p (k n) -> p k n", k=8)
                    udst = wflat[:, 2048:4096].rearrange("p (k n) -> p k n", k=8)
                    if conv:
                        gsrc = wgb[e_, fb].rearrange("p (k n) -> p k n", k=8)
                        usrc = wub[e_, fb].rearrange("p (k n) -> p k n", k=8)
                        dsrc = wdb[e_, fb].rearrange("p (k n) -> p k n", k=2)
                    else:
                        gsrc = w_gate[e_, :, fb * 256:(fb + 1) * 256].rearrange("(k p) n -> p k n", p=128)
                        usrc = w_up[e_, :, fb * 256:(fb + 1) * 256].rearrange("(k p) n -> p k n", p=128)
                        dsrc = w_down[e_, fb * 256:(fb + 1) * 256, :].rearrange("(k p) n -> p k n", p=128)
                    S.op("pool", lambda e: e.dma_start(out=gdst, in_=gsrc), r=[("cg", e_, fb)] if conv else [],
                         w=[("wg", s)], dma=True)
                    S.op("pool", lambda e: e.dma_start(out=udst, in_=usrc), r=[("cu", e_, fb)] if conv else [],
                         w=[("wu", s)], dma=True)
                    S.op("pool", lambda e: e.dma_start(out=wdl[s][:], in_=dsrc), r=[("cd", e_, fb)] if conv else [],
                         w=[("wd", s)], dma=True)
                    for fl in range(2):
                        fc = fb * 2 + fl
                        bk = 4 + fc % 2
                        for k in range(8):
                            S.op("pe", lambda e, k=k: e.matmul(ps[:, bk, 0:CAP], lhsT=wflat[:, k * 256 + fl * 128:k * 256 + (fl + 1) * 128],
                                                               rhs=xsT[:, k, :], start=(k == 0), stop=(k == 7)),
                                 r=[("wg", s), ("xsT", si)], w=PB(bk))
                        for k in range(8):
                            S.op("pe", lambda e, k=k: e.matmul(ps[:, bk, CAP:2 * CAP],
                                                               lhsT=wflat[:, 2048 + k * 256 + fl * 128:2048 + k * 256 + (fl + 1) * 128],
                                                               rhs=xsT[:, k, :], start=(k == 0), stop=(k == 7)),
                                 r=[("wu", s), ("xsT", si)], w=PB(bk), sig=(k == 7))
                        if pend is not None:
                            pend()
                        if fc == 2 and e_ + 1 < E:
                            slot_index(e_ + 1)
                        if fc == 4 and e_ >= 1:
                            scatter(e_ - 1)
                        if fc == 6 and e_ + 1 < E:
                            gather(e_ + 1)
                        if fc == 9 and e_ + 2 < E:
                            build_S(e_ + 2)
                        if fc == 12 and e_ + 1 < E:
                            transposes(e_ + 1)
                        S.op("act", lambda e: e.activation(out=sa[:, 0:CAP], in_=ps[:, bk, 0:CAP], func=AF.Silu), r=PB(bk), w=["sa"])
                        S.op("dve", lambda e: e.tensor_tensor(out=h1T[:, fc, :], in0=ps[:, bk, CAP:2 * CAP], in1=sa[:, 0:CAP],
                                                              op=ALU.mult), r=PB(bk) + ["sa"], w=[("h1T", fc)])

                        def down(fc=fc, s=s, fl=fl):
                            for cc in range(2):
                                for dh in range(2):
                                    S.op("pe", lambda e: e.matmul(ps[:, cc * 2 + dh, :], lhsT=h1T[:, fc, cc * 128:(cc + 1) * 128],
                                                                  rhs=wdl[s][:, fl, dh * 512:(dh + 1) * 512],
                                                                  start=(fc == 0), stop=(fc == 15)),
                                         r=[("h1T", fc), ("wd", s)], w=PB(cc * 2 + dh),
                                         sig=(cc == 1 and dh == 1))
                        pend = down
                pend()
                for cc in range(2):
                    S.op("dve", lambda e: e.scalar_tensor_tensor(out=ysls[si][:, cc, :],
                                                                 in0=ps[:, 2 * cc:2 * cc + 2, :].rearrange("p b c -> p (b c)"),
                                                                 scalar=gsl[:, si, cc:cc + 1], in1=g2fg[:, 0, :], op0=ALU.mult,
                                                                 op1=ALU.mult),
                         r=PB(2 * cc, 2 * cc + 1) + [("gsl", si)], w=[("ysl", cc)])
            scatter(E - 1)
            S.barrier()

        outt = [sb(f"outt{i}", [128, D], F32) for i in range(2)]
        acct = [sb(f"acct{i}", [128, D], F32) for i in range(2)]
        sq3 = sb("sq3", [128, D], BF16)
        def j_A(j):
            s = j % 2
            S.op("sp", lambda e: e.dma_start(out=acct[s][:], in_=accd[j * 128:(j + 1) * 128, :]), w=[("acct", s)], dma=True)
            S.op("dve", lambda e: e.tensor_tensor(out=X[:, j, :], in0=X[:, j, :], in1=acct[s][:], op=ALU.add),
                 r=[("acct", s)], w=[("X", j)])
            if dbg and "x2" in dbg:
                dump2("x2", X[:, j, :], j * 128, D)
            i = 40 + j
            S.op("act", lambda e: e.activation(out=sq3[:], in_=X[:, j, :], func=AF.Square, accum_out=ssq[:, i:i + 1]),
                 r=[("X", j)], w=["sq", ("ssq", i)])
            S.op("act", lambda e: e.activation(out=rstd[:, i:i + 1], in_=ssq[:, i:i + 1], func=AF.Sqrt,
                                               bias=epsb[:, 0:1], scale=1.0 / D), r=[("ssq", i)], w=[("rstd", i)])

        def j_B(j):
            s = j % 2
            i = 40 + j
            S.op("dve", lambda e: e.reciprocal(out=rstd[:, i:i + 1], in_=rstd[:, i:i + 1]), r=[("rstd", i)],
                 w=[("rstd", i)])
            S.op("dve", lambda e: e.scalar_tensor_tensor(out=outt[s][:], in0=X[:, j, :], scalar=rstd[:, i:i + 1],
                                                         in1=g2fg[:, 1, :], op0=ALU.mult, op1=ALU.mult),
                 r=[("X", j), ("rstd", i)], w=[("outt", s)])
            S.op("sp", lambda e: e.dma_start(out=out[j * 128:(j + 1) * 128, :], in_=outt[s][:]), r=[("outt", s)],
                 w=[("out", j)], dma=True)

        j_A(0)
        for j in range(NT):
            if j + 1 < NT:
                j_A(j + 1)
            j_B(j)
        S.finish()
    return nc


def make_consts():
    p = np.arange(128)
    d = p % 64
    axis = d // 32
    half = (d % 32) // 16
    jj = d % 16
    inv = 10000.0 ** (-(np.arange(0, 32, 2, dtype=np.float32)) / 32.0)
    n = np.arange(N)
    pos = np.stack([(n // 64).astype(np.float32), (n % 64).astype(np.float32)], 0)
    ang = pos[axis, :] * inv[jj][:, None].astype(np.float32)
    cos = np.cos(ang).astype(np.float32)
    sin = np.sin(ang).astype(np.float32)
    sgn = np.where(half == 0, -1.0, 1.0).astype(np.float32)[:, None]
    perm = np.zeros((128, 128), np.float32)
    perm[p ^ 16, p] = 1.0
    return np.concatenate([cos, sin * sgn, perm], axis=1).astype(np.float32)


def make_in_maps(inputs, cores):
    cstv = make_consts()
    f = lambda a: np.ascontiguousarray(np.asarray(a, dtype=np.float32))
    shared = {
        "c_ctx": f(inputs["c_ctx"]).reshape(1, D),
        "norm1_g": f(inputs["norm1_g"]).reshape(1, D),
        "norm2_g": f(inputs["norm2_g"]).reshape(1, D),
        "w_ada": f(inputs["w_ada"]).reshape(D, 6 * D),
        "b_ada": f(inputs["b_ada"]).reshape(1, 6 * D),
        "w_in": f(inputs["w_in"]).reshape(D, 8 * D),
        "conv_w": f(inputs["conv_w"]).reshape(3, D),
        "w_out_conv": f(inputs["w_out_conv"]).reshape(D, D),
        "lambda_q1": f(inputs["lambda_q1"]).reshape(1, 64),
        "lambda_k1": f(inputs["lambda_k1"]).reshape(1, 64),
        "lambda_q2": f(inputs["lambda_q2"]).reshape(1, 64),
        "lambda_k2": f(inputs["lambda_k2"]).reshape(1, 64),
        "subln_g": f(inputs["subln_g"]).reshape(1, 128),
        "w_o_attn": f(inputs["w_o_attn"]).reshape(D, D),
        "w_out": f(inputs["w_out"]).reshape(D, D),
        "w_router": f(inputs["w_router"]).reshape(D, E),
        "w_gate_e": f(inputs["w_gate_e"]).reshape(E, D, FH),
        "w_up_e": f(inputs["w_up_e"]).reshape(E, D, FH),
        "w_down_e": f(inputs["w_down_e"]).reshape(E, FH, D),
        "final_g": f(inputs["final_g"]).reshape(1, D),
        "cst": cstv,
    }
    maps = []
    for b in cores:
        m = dict(shared)
        m["x"] = f(inputs["x"][b])
        m["c"] = f(inputs["c"][b]).reshape(1, D)
        m["ctx"] = f(inputs["ctx"][b])
        maps.append(m)
    return maps


def kernel(**inputs):
    nc = build_nc()
    in_maps = make_in_maps(inputs, list(range(8)))
    res = run_bass_kernel_spmd(nc, in_maps, core_ids=list(range(8)))
    return np.stack([np.asarray(r["out"], dtype=np.float32) for r in res.results], axis=0)
```
